# Optimizing a Trainium2 kernel written in Bass

```python
import math
import jax, jax.numpy as jnp
from jax import lax
import numpy as np

D_MODEL = 2048
BATCH = 2
SEQ = 4096
DEPTH = 4

HEAD_DIM = 64
RWKV_HEADS = 12
ATTN_HEADS = 12
GMLP_GROUPS = 8
RWKV_WIDTH = RWKV_HEADS * HEAD_DIM
ATTN_WIDTH = ATTN_HEADS * HEAD_DIM
GMLP_WIDTH = GMLP_GROUPS * HEAD_DIM
MIX_WIDTH = RWKV_WIDTH + ATTN_WIDTH + GMLP_WIDTH
DECAY_LORA = 64
AICL_LORA = 64
GATE_LORA = 128
RWKV_COLS = 3 * RWKV_WIDTH + DECAY_LORA + AICL_LORA + GATE_LORA
ATTN_COLS = 3 * ATTN_WIDTH
GMLP_COLS = 2 * GMLP_WIDTH
IN_COLS = RWKV_COLS + ATTN_COLS + GMLP_COLS
DIL_PAIRS = ((128, 1), (512, 4), (2048, 16))
Q_BLOCK = 128
ROPE_THETA = 500000.0
ROT_DIM = HEAD_DIM // 4
GMLP_CHUNK = 128
D_FF = 5632
CONV_WIDTH = 3
NORM_EPS = 1e-6
GN_EPS = 1e-5
RWKV_LN_EPS = 64e-5
DECAY_SCALE = math.exp(-0.5)
NEG_INF = -1e30

kernel_name = "hybrid_rwkv7_dilated_attn_gmlp_trunk"


def rms_norm(x, g):
    xf = x.astype(jnp.float32)
    y = xf * lax.rsqrt(jnp.mean(xf * xf, axis=-1, keepdims=True) + NORM_EPS)
    return (y * g.astype(jnp.float32)).astype(x.dtype)


def group_norm(x, g, b, eps):
    xf = x.astype(jnp.float32)
    mu = jnp.mean(xf, axis=-1, keepdims=True)
    var = jnp.mean(jnp.square(xf - mu), axis=-1, keepdims=True)
    y = ((xf - mu) * lax.rsqrt(var + eps)).reshape(*x.shape[:-2], -1)
    return y * g.astype(jnp.float32) + b.astype(jnp.float32)


def token_shift(p, mu):
    prev = jnp.pad(p, ((0, 0), (1, 0), (0, 0)))[:, :-1]
    return p + (prev - p) * mu


def rope_partial(x, positions):
    half = ROT_DIM // 2
    inv = ROPE_THETA ** (-jnp.arange(0, ROT_DIM, 2, dtype=jnp.float32) / ROT_DIM)
    ang = positions.astype(jnp.float32)[..., None] * inv
    cos = jnp.cos(ang)[:, :, None, :]
    sin = jnp.sin(ang)[:, :, None, :]
    x1 = x[..., :half].astype(jnp.float32)
    x2 = x[..., half:ROT_DIM].astype(jnp.float32)
    rot = jnp.concatenate([x1 * cos - x2 * sin, x2 * cos + x1 * sin], axis=-1)
    return jnp.concatenate([rot.astype(x.dtype), x[..., ROT_DIM:]], axis=-1)


def rwkv7_mix(p, mu, w0, w2, a0, a2, g2, k_k, k_a, r_k, ln_g, ln_b):
    B, S, _ = p.shape
    H, N = RWKV_HEADS, HEAD_DIM
    f32 = jnp.float32
    p = token_shift(p, mu)
    cuts = [RWKV_WIDTH, 2 * RWKV_WIDTH, 3 * RWKV_WIDTH, 3 * RWKV_WIDTH + DECAY_LORA,
            3 * RWKV_WIDTH + DECAY_LORA + AICL_LORA]
    r, k, v, w_lo, a_lo, g_lo = jnp.split(p, cuts, axis=-1)
    decay = jnp.exp(-DECAY_SCALE * jax.nn.sigmoid((w0 + jnp.tanh(w_lo) @ w2).astype(f32)))
    a = jax.nn.sigmoid((a0 + a_lo @ a2).astype(f32))
    g = jax.nn.sigmoid(g_lo) @ g2
    kk = (k * k_k).reshape(B, S, H, N).astype(f32)
    kk = kk / jnp.maximum(jnp.sqrt(jnp.sum(kk * kk, axis=-1, keepdims=True)), 1e-12)
    k = k.astype(f32) * (1.0 + (a - 1.0) * k_a.astype(f32))
    heads = lambda t: t.reshape(B, S, H, N).astype(f32)
    r_h, w_h, k_h, v_h, a_h = heads(r), heads(decay), heads(k), heads(v), heads(a)

    def step(state, inp):
        r_t, w_t, k_t, v_t, kk_t, a_t = inp
        sk = jnp.einsum('bhvk,bhk->bhv', state, kk_t)
        state = (state * w_t[:, :, None, :]
                 - sk[..., None] * (kk_t * a_t)[:, :, None, :]
                 + v_t[..., None] * k_t[:, :, None, :])
        return state, jnp.einsum('bhvk,bhk->bhv', state, r_t)

    seq_first = lambda t: jnp.moveaxis(t, 1, 0)
    s0 = jnp.zeros((B, H, N, N), f32)
    _, y = lax.scan(step, s0, tuple(seq_first(t) for t in (r_h, w_h, k_h, v_h, kk, a_h)))
    y = jnp.moveaxis(y, 0, 1)
    y = group_norm(y, ln_g, ln_b, RWKV_LN_EPS)
    bonus = jnp.sum(r_h * k_h * r_k.astype(f32), axis=-1, keepdims=True) * v_h
    y = y + bonus.reshape(B, S, H * N)
    return (y * g.astype(f32)).astype(p.dtype)


def dilated_attention(qkv, positions, norm_g):
    B, S, _ = qkv.shape
    H, hd = ATTN_HEADS, HEAD_DIM
    f32 = jnp.float32
    q, k, v = [t.reshape(B, S, H, hd) for t in jnp.split(qkv, 3, axis=-1)]
    q = rope_partial(q, positions)
    k = rope_partial(k, positions)
    scale = HEAD_DIM ** -0.5
    n_blocks = S // Q_BLOCK

    def block(bi):
        start = bi * Q_BLOCK
        t = start + jnp.arange(Q_BLOCK)
        q_b = lax.dynamic_slice_in_dim(q, start, Q_BLOCK, axis=1).astype(f32)
        outs, lses = [], []
        for win, dil in DIL_PAIRS:
            offs = dil * jnp.arange(win // dil + 1)
            idx = t[:, None] - offs[None, :]
            valid = idx >= 0
            idx = jnp.maximum(idx, 0)
            k_g = k[:, idx].astype(f32)
            v_g = v[:, idx].astype(f32)
            s = jnp.einsum('bqhd,bqkhd->bhqk', q_b, k_g) * scale
            s = jnp.where(valid[None, None], s, NEG_INF)
            m = jnp.max(s, axis=-1, keepdims=True)
            pr = jnp.exp(s - m)
            l = jnp.sum(pr, axis=-1, keepdims=True)
            outs.append(jnp.einsum('bhqk,bqkhd->bhqd', pr, v_g) / l)
            lses.append(m + jnp.log(l))
        wts = jax.nn.softmax(jnp.stack(lses), axis=0)
        out = jnp.sum(wts * jnp.stack(outs), axis=0)
        return jnp.swapaxes(out, 1, 2).astype(qkv.dtype)

    o = lax.map(block, jnp.arange(n_blocks))
    o = jnp.moveaxis(o, 0, 1).reshape(B, S, H * hd)
    return rms_norm(o, norm_g)


def chunked_sgu(uv, ln_g, ln_b, ws, bs, norm_g):
    B, S, _ = uv.shape
    G, N, C = GMLP_GROUPS, GMLP_WIDTH // GMLP_GROUPS, GMLP_CHUNK
    z = jax.nn.gelu(uv)
    u, v = jnp.split(z, 2, axis=-1)
    v = group_norm(v.reshape(B, S, G, N), ln_g, ln_b, GN_EPS).astype(uv.dtype)
    v = v.reshape(B, S // C, C, G, N)
    w = jnp.where(jnp.tril(jnp.ones((C, C), dtype=bool))[None], ws, 0.0)
    v = jnp.einsum('gij,bcjgn->bcign', w, v) + bs.T[None, None, :, :, None]
    return rms_norm(u * v.reshape(B, S, GMLP_WIDTH), norm_g)


def conv_ffn(h, w_up, conv_w, conv_b, w_down):
    S = h.shape[1]
    up = h @ w_up
    padded = jnp.pad(up, ((0, 0), (CONV_WIDTH - 1, 0), (0, 0)))
    conv = conv_b + padded[:, 0:S] * conv_w[0]
    for j in range(1, CONV_WIDTH):
        conv = conv + padded[:, j:j + S] * conv_w[j]
    gate, val = jnp.split(conv, 2, axis=-1)
    return (jax.nn.gelu(gate, approximate=True) * val) @ w_down


def setup_inputs(seed: int = 0) -> dict:
    key = jax.random.key(seed)
    keys = jax.random.split(key, 32)
    counter = [0]

    def nxt():
        kk = keys[counter[0]]
        counter[0] += 1
        return kk

    f32 = jnp.float32
    nrm = lambda shape, scale: scale * jax.random.normal(nxt(), shape, f32)
    gain = lambda shape: 1.0 + nrm(shape, 0.05)
    L, D = DEPTH, D_MODEL
    return {
        "x": jax.random.normal(nxt(), (BATCH, SEQ, D), f32),
        "positions": jnp.broadcast_to(jnp.arange(SEQ, dtype=jnp.int32), (BATCH, SEQ)),
        "norm_mix_pre": gain((L, D)),
        "norm_mix_post": gain((L, D)),
        "norm_ffn_pre": gain((L, D)),
        "norm_ffn_post": gain((L, D)),
        "w_in": nrm((L, D, IN_COLS), D ** -0.5),
        "rwkv_mu": jax.random.uniform(nxt(), (L, RWKV_COLS), f32),
        "rwkv_w0": nrm((L, RWKV_WIDTH), 1.0),
        "rwkv_w2": nrm((L, DECAY_LORA, RWKV_WIDTH), DECAY_LORA ** -0.5),
        "rwkv_a0": nrm((L, RWKV_WIDTH), 0.5),
        "rwkv_a2": nrm((L, AICL_LORA, RWKV_WIDTH), AICL_LORA ** -0.5),
        "rwkv_g2": nrm((L, GATE_LORA, RWKV_WIDTH), GATE_LORA ** -0.5),
        "rwkv_k_k": 0.85 + nrm((L, RWKV_WIDTH), 0.05),
        "rwkv_k_a": gain((L, RWKV_WIDTH)),
        "rwkv_r_k": nrm((L, RWKV_HEADS, HEAD_DIM), 0.1),
        "rwkv_ln_g": gain((L, RWKV_WIDTH)),
        "rwkv_ln_b": nrm((L, RWKV_WIDTH), 0.02),
        "attn_norm_g": gain((L, ATTN_WIDTH)),
        "gmlp_ln_g": gain((L, GMLP_WIDTH)),
        "gmlp_ln_b": nrm((L, GMLP_WIDTH), 0.02),
        "gmlp_ws": nrm((L, GMLP_GROUPS, GMLP_CHUNK, GMLP_CHUNK), GMLP_CHUNK ** -0.5),
        "gmlp_bs": 1.0 + nrm((L, GMLP_GROUPS, GMLP_CHUNK), 0.1),
        "gmlp_norm_g": gain((L, GMLP_WIDTH)),
        "w_out": nrm((L, MIX_WIDTH, D), MIX_WIDTH ** -0.5),
        "ffn_up": nrm((L, D, 2 * D_FF), D ** -0.5),
        "ffn_conv_w": nrm((L, CONV_WIDTH, 2 * D_FF), CONV_WIDTH ** -0.5),
        "ffn_conv_b": nrm((L, 2 * D_FF), 0.02),
        "ffn_down": nrm((L, D_FF, D), D_FF ** -0.5),
    }


def reference(x, positions, norm_mix_pre, norm_mix_post, norm_ffn_pre, norm_ffn_post,
              w_in, rwkv_mu, rwkv_w0, rwkv_w2, rwkv_a0, rwkv_a2, rwkv_g2, rwkv_k_k,
              rwkv_k_a, rwkv_r_k, rwkv_ln_g, rwkv_ln_b, attn_norm_g, gmlp_ln_g, gmlp_ln_b,
              gmlp_ws, gmlp_bs, gmlp_norm_g, w_out, ffn_up, ffn_conv_w, ffn_conv_b, ffn_down):
    for l in range(DEPTH):
        hn = rms_norm(x, norm_mix_pre[l])
        proj = hn @ w_in[l]
        p_rwkv, p_attn, p_gmlp = jnp.split(proj, [RWKV_COLS, RWKV_COLS + ATTN_COLS], axis=-1)
        y_a = rwkv7_mix(p_rwkv, rwkv_mu[l], rwkv_w0[l], rwkv_w2[l], rwkv_a0[l], rwkv_a2[l],
                        rwkv_g2[l], rwkv_k_k[l], rwkv_k_a[l], rwkv_r_k[l],
                        rwkv_ln_g[l], rwkv_ln_b[l])
        y_b = dilated_attention(p_attn, positions, attn_norm_g[l])
        y_c = chunked_sgu(p_gmlp, gmlp_ln_g[l], gmlp_ln_b[l], gmlp_ws[l], gmlp_bs[l],
                          gmlp_norm_g[l])
        y = jnp.concatenate([y_a.astype(x.dtype), y_b.astype(x.dtype), y_c.astype(x.dtype)],
                            axis=-1)
        x = x + rms_norm(y @ w_out[l], norm_mix_post[l])
        hn = rms_norm(x, norm_ffn_pre[l])
        x = x + rms_norm(conv_ffn(hn, ffn_up[l], ffn_conv_w[l], ffn_conv_b[l], ffn_down[l]),
                         norm_ffn_post[l])
    return x
```

```python
import numpy as np
import ml_dtypes
from contextlib import ExitStack
import concourse.bass as bass
import concourse.mybir as mybir
from concourse.bass_utils import run_bass_kernel_spmd

F32 = mybir.dt.float32
BF16 = mybir.dt.bfloat16
I32 = mybir.dt.int32
AF = mybir.ActivationFunctionType
ALU = mybir.AluOpType
AX = mybir.AxisListType

D = 2048
DFF = 5632
NORM_EPS = 1e-6

SAME_ENGINE_SYNC = True


class Tok:
    __slots__ = ("owner", "sem", "key", "val")

    def __init__(self, owner, sem, key, val):
        self.owner, self.sem, self.key, self.val = owner, sem, key, val


class Buf:
    __slots__ = ("w", "r")

    def __init__(self):
        self.w = None
        self.r = {}


class V:
    __slots__ = ("ap", "buf")

    def __init__(self, ap, buf):
        self.ap, self.buf = ap, buf

    def __getitem__(self, k):
        return V(self.ap[k], self.buf)

    def re(self, s, **kw):
        return V(self.ap.rearrange(s, **kw), self.buf)

    def bc(self, shape):
        return V(self.ap.to_broadcast(shape), self.buf)

    def bc3(self, n=64):
        sh = list(self.ap.shape)
        return V(self.ap.unsqueeze(2).to_broadcast([sh[0], sh[1], n]), self.buf)


ENGS = {"pe": "tensor", "act": "scalar", "dve": "vector", "pool": "gpsimd", "sp": "sync"}


class Sched:
    def __init__(self, nc, n_dma=48):
        self.nc = nc
        self.e = {k: getattr(nc, v) for k, v in ENGS.items()}
        self.sem = {k: nc.alloc_semaphore("s_" + k) for k in ENGS}
        self.cnt = {k: 0 for k in ENGS}
        self.seen = {k: {} for k in ENGS}
        self.dsem = [nc.alloc_semaphore("d%d" % i) for i in range(n_dma)]
        self.dcnt = [0] * n_dma
        self.dnext = 0
        self.nid = 0
        self.ninst = 0
        for s in list(self.sem.values()) + self.dsem:
            nc.gpsimd.sem_clear(s)
        nc.all_engine_barrier()

    def name(self, p):
        self.nid += 1
        return "%s_%d" % (p, self.nid)

    def sb(self, es, shape, dtype, name="t"):
        t = es.enter_context(self.nc.sbuf_tensor(self.name(name), list(shape), dtype))
        return V(t.ap(), Buf())

    def ps(self, es, shape, dtype=F32, name="p"):
        t = es.enter_context(self.nc.psum_tensor(self.name(name), list(shape), dtype))
        return V(t.ap(), Buf())

    def _wait(self, eng, tok):
        if tok.owner == eng and (eng == "pe" or not SAME_ENGINE_SYNC):
            return
        if self.seen[eng].get(tok.key, 0) >= tok.val:
            return
        self.e[eng].wait_ge(tok.sem, tok.val)
        self.seen[eng][tok.key] = tok.val

    def _deps(self, eng, reads, writes):
        for v in reads:
            if v.buf.w is not None:
                self._wait(eng, v.buf.w)
        for v in writes:
            b = v.buf
            if b.w is not None:
                self._wait(eng, b.w)
            for t in b.r.values():
                self._wait(eng, t)

    def _mark(self, tok, reads, writes):
        for v in reads:
            r = v.buf.r
            o = r.get(tok.key)
            if o is None or o.val < tok.val:
                r[tok.key] = tok
        for v in writes:
            v.buf.w = tok
            v.buf.r = {}

    def I(self, eng, method, inc=True, **kw):
        reads, writes, args = [], [], {}
        for k, a in kw.items():
            if isinstance(a, V):
                (writes if (k.startswith("out") or k == "accum_out" or k == "ap") else reads).append(a)
                args[k] = a.ap
            else:
                args[k] = a
        self._deps(eng, reads, writes)
        ins = getattr(self.e[eng], method)(**args)
        self.ninst += 1
        if inc:
            self.cnt[eng] += 1
            ins.then_inc(self.sem[eng], 1)
            tok = Tok(eng, self.sem[eng], eng, self.cnt[eng])
        else:
            tok = Tok(eng, self.sem[eng], eng, self.cnt[eng] + 1)
        self._mark(tok, reads, writes)
        return ins

    def dma(self, q, out, in_):
        if q == "pool":
            q = "sp"
        i = self.dnext
        self.dnext = (i + 1) % len(self.dsem)
        key = ("d", i)
        if self.dcnt[i] > 0:
            self._wait(q, Tok("dma", self.dsem[i], key, 16 * self.dcnt[i]))
        reads = [in_] if isinstance(in_, V) else []
        writes = [out] if isinstance(out, V) else []
        self._deps(q, reads, writes)
        ins = self.e[q].dma_start(out=out.ap if isinstance(out, V) else out,
                                  in_=in_.ap if isinstance(in_, V) else in_)
        self.ninst += 1
        self.dcnt[i] += 1
        ins.then_inc(self.dsem[i], 16)
        tok = Tok("dma", self.dsem[i], key, 16 * self.dcnt[i])
        self._mark(tok, reads, writes)
        return tok

    def barrier(self):
        toks = [Tok(k, self.sem[k], k, self.cnt[k]) for k in ENGS if self.cnt[k] > 0]
        toks += [Tok("dma", self.dsem[i], ("d", i), 16 * c) for i, c in enumerate(self.dcnt) if c > 0]
        for eng in ENGS:
            for t in toks:
                self._wait(eng, t)

    def finish(self):
        self.barrier()
        self.nc.all_engine_barrier()
        for s in list(self.sem.values()) + self.dsem:
            self.nc.gpsimd.sem_clear(s)
        self.nc.all_engine_barrier()


def rstd_from_ss(S, ss, out, n, eps):
    S.I("act", "activation", out=out, in_=ss, func=AF.Sqrt, scale=1.0 / n, bias=float(eps))
    S.I("dve", "reciprocal", out=out, in_=out)


def transpose_tile(S, src_bf, dst, ptr, ident, nchunks, evac_engs=("dve", "act")):
    k = 0
    for c0 in range(0, nchunks, 4):
        n = min(4, nchunks - c0)
        p = ptr[k % len(ptr)]
        for j in range(n):
            S.I("pe", "transpose", out=p[:, j, :], in_=src_bf[:, (c0 + j) * 128:(c0 + j + 1) * 128], identity=ident)
        eng = evac_engs[k % len(evac_engs)]
        if eng == "act":
            S.I("act", "activation", out=dst[:, c0:c0 + n, :], in_=p[:, 0:n, :], func=AF.Copy)
        else:
            S.I(eng, "tensor_copy", out=dst[:, c0:c0 + n, :], in_=p[:, 0:n, :])
        k += 1


def t_pass(S, es0, C, W, xin, yin, xout, x1_scr, NT, load_x=None, load_y=None, perm=False):
    nc = S.nc
    ntile = NT // 128 + 1
    TC = 128 + NT
    ident = C["ident"]
    with ExitStack() as es:
        hn2T = S.sb(es, [128, 16, TC], BF16, "hn2T")
        with ExitStack() as e1:
            wob = S.sb(e1, [128, 16, D], BF16, "wob")
            wst = [S.sb(e1, [128, D], F32, "wst") for _ in range(2)]
            gB_post = S.sb(e1, [128, D], F32, "gBpost")
            gB_pre2 = S.sb(e1, [128, D], F32, "gBpre2")
            gB_y = S.sb(e1, [128, 1280], F32, "gBy")
            xt = S.sb(e1, [128, D], F32, "xt")
            yc = S.sb(e1, [128, D], F32, "yc")
            ybf = S.sb(e1, [128, D], BF16, "ybf")
            yT = S.sb(e1, [128, 16, 128], BF16, "yT")
            x1t = S.sb(e1, [128, D], F32, "x1t")
            tmp = S.sb(e1, [128, D], F32, "tmp")
            hbf = S.sb(e1, [128, D], BF16, "hbf")
            junk = S.sb(e1, [128, D], BF16, "junk")
            st = S.sb(e1, [128, 16], F32, "st")
            ptr = [S.ps(e1, [128, 4, 128], BF16, "ptr") for _ in range(2)]
            pz = [S.ps(e1, [128, 512], F32, "pz") for _ in range(4)]
            S.dma("pool", gB_post, W["g_post"].partition_broadcast(128))
            S.dma("pool", gB_pre2, W["g_pre2"].partition_broadcast(128))
            S.dma("pool", gB_y[:, 0:768], W["attn_g"].partition_broadcast(128))
            S.dma("pool", gB_y[:, 768:1280], W["gmlp_g"].partition_broadcast(128))
            wo = W["w_out"].rearrange("(c p) n -> c p n", p=128)
            for c in range(16):
                S.dma("sp", wst[c % 2], wo[c])
                S.I("pool", "tensor_copy", out=wob[:, c, :], in_=wst[c % 2])
            for i in range(ntile):
                r0 = i * 128
                if load_x is None:
                    S.dma("sp", xt, xin[r0:r0 + 128, :])
                else:
                    load_x(i, xt, (tmp, x1t))
                if load_y is None:
                    S.dma("sp", yc, yin[r0:r0 + 128, :])
                else:
                    load_y(i, yc, (tmp, x1t))
                if perm:
                    v4 = lambda t_: t_.re("p (r c) -> p r c", r=4)
                    ya_v, ob_v, uc_v = v4(yc)[:, :, 0:192], v4(yc)[:, :, 192:384], v4(yc)[:, :, 384:512]
                    ya_o, ob_o, uc_o = v4(ybf)[:, :, 0:192], v4(ybf)[:, :, 192:384], v4(ybf)[:, :, 384:512]
                    jk_b, jk_c = v4(junk)[:, :, 192:384], v4(junk)[:, :, 384:512]
                    gA, gG = v4(gB_y[:, 0:768]), v4(gB_y[:, 768:1280])
                else:
                    ya_v, ob_v, uc_v = yc[:, 0:768], yc[:, 768:1536], yc[:, 1536:2048]
                    ya_o, ob_o, uc_o = ybf[:, 0:768], ybf[:, 768:1536], ybf[:, 1536:2048]
                    jk_b, jk_c = junk[:, 0:768], junk[:, 768:1280]
                    gA, gG = gB_y[:, 0:768], gB_y[:, 768:1280]
                S.I("act", "activation", out=jk_b, in_=ob_v, func=AF.Square, accum_out=st[:, 0:1])
                S.I("act", "activation", out=jk_c, in_=uc_v, func=AF.Square, accum_out=st[:, 1:2])
                rstd_from_ss(S, st[:, 0:1], st[:, 2:3], 768, NORM_EPS)
                rstd_from_ss(S, st[:, 1:2], st[:, 3:4], 512, NORM_EPS)
                S.I("pool", "tensor_copy", out=ya_o, in_=ya_v)
                S.I("dve", "scalar_tensor_tensor", out=ob_o, in0=ob_v, scalar=st[:, 2:3],
                    in1=gA, op0=ALU.mult, op1=ALU.mult)
                S.I("dve", "scalar_tensor_tensor", out=uc_o, in0=uc_v, scalar=st[:, 3:4],
                    in1=gG, op0=ALU.mult, op1=ALU.mult)
                transpose_tile(S, ybf, yT, ptr, ident, 16)
                for cb in range(4):
                    for c in range(16):
                        S.I("pe", "matmul", inc=(c == 15), out=pz[cb], lhsT=yT[:, c, :],
                            rhs=wob[:, c, cb * 512:(cb + 1) * 512], start=(c == 0), stop=(c == 15))
                    S.I("act", "activation", out=junk[:, cb * 512:(cb + 1) * 512], in_=pz[cb], func=AF.Square,
                        accum_out=st[:, 4 + cb:5 + cb])
                S.I("dve", "reduce_sum", out=st[:, 8:9], in_=st[:, 4:8], axis=AX.X)
                rstd_from_ss(S, st[:, 8:9], st[:, 9:10], D, NORM_EPS)
                for cb in range(4):
                    sl = slice(cb * 512, (cb + 1) * 512)
                    S.I("dve", "scalar_tensor_tensor", out=tmp[:, sl], in0=pz[cb], scalar=st[:, 9:10],
                        in1=gB_post[:, sl], op0=ALU.mult, op1=ALU.mult)
                    S.I("pool", "tensor_tensor", out=x1t[:, sl], in0=tmp[:, sl], in1=xt[:, sl], op=ALU.add)
                if i > 0:
                    S.dma("pool", x1_scr[r0 - 128:r0, :], x1t)
                S.I("act", "activation", out=junk, in_=x1t, func=AF.Square, accum_out=st[:, 10:11])
                rstd_from_ss(S, st[:, 10:11], st[:, 11:12], D, NORM_EPS)
                S.I("dve", "scalar_tensor_tensor", out=hbf, in0=x1t, scalar=st[:, 11:12], in1=gB_pre2,
                    op0=ALU.mult, op1=ALU.mult)
                transpose_tile(S, hbf, hn2T[:, :, r0:r0 + 128], ptr, ident, 16)
        S.barrier()
        actT = S.sb(es, [128, 44, NT], BF16, "actT")
        cwb = S.sb(es, [128, 88, 4], F32, "cwb")
        S.dma("pool", cwb, W["cwb"])
        NU = NT + 2
        with ExitStack() as e2:
            wst = [S.sb(e2, [128, 16, 128], F32, "wst2") for _ in range(3)]
            wbf = [S.sb(e2, [128, 16, 128], BF16, "wbf2") for _ in range(3)]
            ups = [S.sb(e2, [128, NU], F32, "ups") for _ in range(2)]
            cv = [S.sb(e2, [128, NT], F32, "cv") for _ in range(2)]
            tmpc = S.sb(e2, [128, NT], F32, "tmpc")
            ggl = S.sb(e2, [128, NT], F32, "ggl")
            pu = [S.ps(e2, [128, 1024], F32, "pu") for _ in range(2)]
            wu = W["ffn_up"].rearrange("(c p) f -> p c f", p=128)
            blocks = []
            o = 0
            while o < NU:
                n = min(512, NU - o)
                blocks.append((o, n))
                o += n
            k = 0
            for fb in range(44):
                for half in range(2):
                    col0 = half * DFF + fb * 128
                    fb2 = half * 44 + fb
                    S.dma("sp", wst[k % 3], wu[:, :, col0:col0 + 128])
                    S.I("act", "activation", out=wbf[k % 3], in_=wst[k % 3], func=AF.Copy)
                    p = pu[half]
                    for (o, n) in blocks:
                        for c in range(16):
                            S.I("pe", "matmul", inc=(c == 15), out=p[:, o:o + n], lhsT=wbf[k % 3][:, c, :],
                                rhs=hn2T[:, c, 126 + o:126 + o + n], start=(c == 0), stop=(c == 15))
                    u = ups[half]
                    S.I("act", "activation", out=u, in_=p[:, 0:NU], func=AF.Copy)
                    w0, w1, w2, bb = (cwb[:, fb2, j:j + 1] for j in range(4))
                    if half == 0:
                        S.I("dve", "tensor_scalar", out=cv[0], in0=u[:, 2:NU], scalar1=w2, scalar2=bb,
                            op0=ALU.mult, op1=ALU.add)
                        S.I("dve", "scalar_tensor_tensor", out=cv[0], in0=u[:, 1:NU - 1], scalar=w1, in1=cv[0],
                            op0=ALU.mult, op1=ALU.add)
                        S.I("dve", "scalar_tensor_tensor", out=cv[0], in0=u[:, 0:NU - 2], scalar=w0, in1=cv[0],
                            op0=ALU.mult, op1=ALU.add)
                        S.I("act", "activation", out=ggl, in_=cv[0], func=AF.Gelu_apprx_tanh)
                    else:
                        S.I("pool", "tensor_scalar", out=cv[1], in0=u[:, 2:NU], scalar1=w2, scalar2=bb,
                            op0=ALU.mult, op1=ALU.add)
                        S.I("pool", "tensor_scalar", out=tmpc, in0=u[:, 1:NU - 1], scalar1=w1, scalar2=None,
                            op0=ALU.mult)
                        S.I("pool", "tensor_tensor", out=cv[1], in0=cv[1], in1=tmpc, op=ALU.add)
                        S.I("pool", "tensor_scalar", out=tmpc, in0=u[:, 0:NU - 2], scalar1=w0, scalar2=None,
                            op0=ALU.mult)
                        S.I("pool", "tensor_tensor", out=cv[1], in0=cv[1], in1=tmpc, op=ALU.add)
                        S.I("dve", "tensor_tensor", out=actT[:, fb, :], in0=ggl, in1=cv[1], op=ALU.mult)
                    k += 1
        S.barrier()
        with ExitStack() as e3:
            nt = NT // 128
            acc = S.sb(e3, [128, nt, D], F32, "acc")
            wst = [S.sb(e3, [128, D], F32, "wst3") for _ in range(2)]
            wdb = [S.sb(e3, [128, D], BF16, "wdb") for _ in range(8)]
            gB_post2 = S.sb(e3, [128, D], F32, "gBpost2")
            x1t = S.sb(e3, [128, D], F32, "x1t3")
            ot = S.sb(e3, [128, D], F32, "ot")
            junk = S.sb(e3, [128, D], BF16, "junk3")
            st = S.sb(e3, [128, 4], F32, "st3")
            pd = [S.ps(e3, [128, 512], F32, "pd") for _ in range(8)]
            S.dma("pool", gB_post2, W["g_post2"].partition_broadcast(128))
            wd = W["ffn_down"].rearrange("(c p) n -> c p n", p=128)
            G = 4
            k = 0
            for g in range(44 // G):
                for j in range(G):
                    fb = g * G + j
                    S.dma("sp", wst[fb % 2], wd[fb])
                    S.I("pool", "tensor_copy", out=wdb[fb % 8], in_=wst[fb % 2])
                for i in range(nt):
                    for cb in range(4):
                        p = pd[k % 8]
                        k += 1
                        for j in range(G):
                            fb = g * G + j
                            S.I("pe", "matmul", inc=(j == G - 1), out=p, lhsT=actT[:, fb, i * 128:(i + 1) * 128],
                                rhs=wdb[fb % 8][:, cb * 512:(cb + 1) * 512], start=(j == 0), stop=(j == G - 1))
                        sl = slice(cb * 512, (cb + 1) * 512)
                        if g == 0:
                            S.I("act", "activation", out=acc[:, i, sl], in_=p, func=AF.Copy)
                        else:
                            S.I("dve", "tensor_tensor", out=acc[:, i, sl], in0=p, in1=acc[:, i, sl], op=ALU.add)
            for i in range(nt):
                S.dma("sp", x1t, x1_scr[i * 128:(i + 1) * 128, :])
                S.I("act", "activation", out=junk, in_=acc[:, i, :], func=AF.Square, accum_out=st[:, 0:1])
                rstd_from_ss(S, st[:, 0:1], st[:, 1:2], D, NORM_EPS)
                S.I("dve", "scalar_tensor_tensor", out=ot, in0=acc[:, i, :], scalar=st[:, 1:2], in1=gB_post2,
                    op0=ALU.mult, op1=ALU.mult)
                S.I("pool", "tensor_tensor", out=ot, in0=ot, in1=x1t, op=ALU.add)
                S.dma("pool", xout[i * 128:(i + 1) * 128, :], ot)
        S.barrier()


def build_T(NT=512, npass=2):
    nc = bass.Bass("TRN2", target_bir_lowering=False)
    TC = 128 + NT
    xin = nc.dram_tensor("xin", [npass, TC, D], F32, kind="ExternalInput").ap()
    yin = nc.dram_tensor("yin", [npass, TC, D], F32, kind="ExternalInput").ap()
    W = {
        "w_out": nc.dram_tensor("w_out", [D, D], F32, kind="ExternalInput").ap(),
        "ffn_up": nc.dram_tensor("ffn_up", [D, 2 * DFF], F32, kind="ExternalInput").ap(),
        "ffn_down": nc.dram_tensor("ffn_down", [DFF, D], F32, kind="ExternalInput").ap(),
        "cwb": nc.dram_tensor("cwb", [128, 88, 4], F32, kind="ExternalInput").ap(),
        "g_post": nc.dram_tensor("g_post", [D], F32, kind="ExternalInput").ap(),
        "g_pre2": nc.dram_tensor("g_pre2", [D], F32, kind="ExternalInput").ap(),
        "g_post2": nc.dram_tensor("g_post2", [D], F32, kind="ExternalInput").ap(),
        "attn_g": nc.dram_tensor("attn_g", [768], F32, kind="ExternalInput").ap(),
        "gmlp_g": nc.dram_tensor("gmlp_g", [512], F32, kind="ExternalInput").ap(),
    }
    ident_d = nc.dram_tensor("ident", [128, 128], BF16, kind="ExternalInput").ap()
    xout = nc.dram_tensor("xout", [npass, NT, D], F32, kind="ExternalOutput").ap()
    x1_scr = nc.dram_tensor("x1_scr", [NT, D], F32, kind="Internal").ap()
    S = Sched(nc)
    with ExitStack() as es0:
        ident = S.sb(es0, [128, 128], BF16, "ident")
        S.dma("sp", ident, ident_d)
        C = {"ident": ident}
        for ps_ in range(npass):
            t_pass(S, es0, C, W, xin[ps_], yin[ps_], xout[ps_], x1_scr, NT)
        S.finish()
    return nc, S


def t_weights(inp, l):
    cw = np.asarray(inp["ffn_conv_w"][l], np.float32)
    cb = np.asarray(inp["ffn_conv_b"][l], np.float32)
    cwb = np.concatenate([cw, cb[None]], 0)
    cwb = np.ascontiguousarray(cwb.reshape(4, 88, 128).transpose(2, 1, 0))
    return {
        "w_out": np.ascontiguousarray(inp["w_out"][l]), "ffn_up": np.ascontiguousarray(inp["ffn_up"][l]),
        "ffn_down": np.ascontiguousarray(inp["ffn_down"][l]), "cwb": cwb,
        "g_post": np.ascontiguousarray(inp["norm_mix_post"][l]), "g_pre2": np.ascontiguousarray(inp["norm_ffn_pre"][l]),
        "g_post2": np.ascontiguousarray(inp["norm_ffn_post"][l]),
        "attn_g": np.ascontiguousarray(inp["attn_norm_g"][l]), "gmlp_g": np.ascontiguousarray(inp["gmlp_norm_g"][l]),
        "ident": np.eye(128, dtype=ml_dtypes.bfloat16),
    }


def t_shard(full, b, t0, NT, npass):
    out = np.zeros((npass, 128 + NT, full.shape[-1]), np.float32)
    for p in range(npass):
        s = t0 + p * NT
        out[p, 128:] = full[b, s:s + NT]
        if s > 0:
            out[p, :128] = full[b, s - 128:s]
    return out


SEQ = 4096
DECAY_SCALE = float(np.exp(-0.5))
TWO_PI = float(2.0 * np.pi)


def _tt(S, eng, out, a, b, op):
    S.I(eng, "tensor_tensor", out=out, in0=a, in1=b, op=op)


def _ts(S, eng, out, a, s1, op0, s2=None, op1=None):
    kw = dict(out=out, in0=a, scalar1=s1, scalar2=s2, op0=op0)
    if op1 is not None:
        kw["op1"] = op1
    S.I(eng, "tensor_scalar", **kw)


def _stt(S, out, a, s, b, op0, op1):
    S.I("dve", "scalar_tensor_tensor", out=out, in0=a, scalar=s, in1=b, op0=op0, op1=op1)


def _act(S, out, in_, func, **kw):
    S.I("act", "activation", out=out, in_=in_, func=func, **kw)


def m_consts_dev(S, es, A):
    MUL, ADD, SUB, MAX, MIN = ALU.mult, ALU.add, ALU.subtract, ALU.max, ALU.min
    Cst = {}
    ident = S.sb(es, [128, 128], BF16, "ident")
    identf = S.sb(es, [128, 128], F32, "identf")
    amask = S.sb(es, [128, 17, 128], BF16, "amask")
    rmask = S.sb(es, [64, 448], F32, "rmask")
    tril = S.sb(es, [128, 128], F32, "tril")
    S.dma("sp", ident, A["ident"])
    S.dma("sp", identf, A["identf"])
    S.dma("sp", amask, A["amask"])
    S.dma("sp", rmask, A["rmask"])
    S.dma("sp", tril, A["tril"])
    ones64f = S.sb(es, [64, 64], F32, "ones64f")
    S.I("dve", "memset", ap=ones64f, constant=1.0)
    ones3 = S.sb(es, [3, 128], F32, "ones3")
    S.I("dve", "memset", ap=ones3, constant=1.0)
    cs = S.sb(es, [128, 32, 16], F32, "cs")
    with ExitStack() as e0:
        posi = S.sb(e0, [128, 32], I32, "posi")
        posf = S.sb(e0, [128, 32], F32, "posf")
        ang = S.sb(e0, [128, 32, 16], F32, "ang")
        kf = S.sb(e0, [128, 32, 16], F32, "kf")
        ki = S.sb(e0, [128, 32, 16], I32, "ki")
        S.dma("sp", posi, A["pos"])
        S.I("dve", "tensor_copy", out=posf, in_=posi)
        for i in range(8):
            inv = float(500000.0 ** (-(2.0 * i) / 16.0))
            _ts(S, "dve", ang[:, :, 8 + i], posf, inv, MUL)
            _ts(S, "dve", ang[:, :, i], posf, inv, MUL, float(np.pi / 2), ADD)
        _ts(S, "dve", kf, ang, 1.0 / TWO_PI, MUL)
        S.I("dve", "tensor_copy", out=ki, in_=kf)
        S.I("dve", "tensor_copy", out=kf, in_=ki)
        _stt(S, ang, kf, -TWO_PI, ang, MUL, ADD)
        _ts(S, "dve", ang, ang, float(np.pi), MIN, float(-np.pi), MAX)
        _act(S, cs, ang, AF.Sin)
    S.barrier()
    Cst.update(ident=ident, identf=identf, amask=amask, rmask=rmask, tril=tril, ones64f=ones64f, ones3=ones3, cs=cs)
    return Cst


def m_phase(S, es, Cst, A, NBLK=16, BT=256, store=None):
    nc = S.nc
    NCH, NTI = BT // 64, BT // 128
    MUL, ADD, SUB, MAX, MIN = ALU.mult, ALU.add, ALU.subtract, ALU.max, ALU.min
    if store is None:
        def store(kind, t0, n, tile):
            S.dma("sp", A[kind][t0:t0 + n, :], tile)
    ident, identf, amask, rmask, tril = Cst["ident"], Cst["identf"], Cst["amask"], Cst["rmask"], Cst["tril"]
    ones64f, ones3, cs = Cst["ones64f"], Cst["ones3"], Cst["cs"]
    B_pf = S.ps(es, [128, 512], F32, "b_pf")
    B_g1 = S.ps(es, [128, 512], F32, "b_g1")
    B_g2 = S.ps(es, [128, 512], F32, "b_g2")
    B_tr = S.ps(es, [128, 1024], BF16, "b_tr")
    B_s = S.ps(es, [128, 512], F32, "b_s")
    B_o = S.ps(es, [128, 512], F32, "b_o")
    R0 = S.ps(es, [128, 512], F32, "r0")
    R1 = S.ps(es, [128, 512], F32, "r1")

    maskN = rmask[:, 0:128]
    maskP = rmask[:, 128:256]
    maskL = rmask[:, 256:320]

    gT = S.sb(es, [128, 16], F32, "gT")
    S.dma("sp", gT, A["gT"])
    wfm = S.sb(es, [128, 16, 832], BF16, "wfm")
    wtm = S.sb(es, [128, 16, 832], BF16, "wtm")
    with ExitStack() as e0:
        stg = [S.sb(e0, [128, 832], F32, "stg") for _ in range(2)]
        k = 0
        for (dst, src) in ((wfm, A["w_fm"]), (wtm, A["w_tm"])):
            sv = src.rearrange("(c p) n -> c p n", p=128)
            for c in range(16):
                S.dma("sp", stg[k % 2], sv[c])
                _ts(S, "pool" if k % 2 else "dve", dst[:, c, :], stg[k % 2], gT[:, c:c + 1], MUL)
                k += 1
    S.barrier()
    mu = S.sb(es, [128, 12], F32, "mu")
    S.dma("sp", mu, A["mu_fm"])
    vec = S.sb(es, [64, 18], F32, "vec")
    S.dma("sp", vec[:, 0:15], A["vec"])
    for h in range(3):
        _ts(S, "dve", vec[:, 15 + h:16 + h], vec[:, 5 * h + 3:5 * h + 4], -1.0, MUL, 1.0, ADD)
    rkb = S.sb(es, [64, 3], BF16, "rkb")
    for h in range(3):
        S.I("dve", "tensor_copy", out=rkb[:, h:h + 1], in_=vec[:, 5 * h + 4:5 * h + 5])
    w2b = S.sb(es, [64, 192], BF16, "w2b")
    a2b = S.sb(es, [64, 192], BF16, "a2b")
    g2b = S.sb(es, [128, 192], BF16, "g2b")
    lnG = S.sb(es, [64, 192], F32, "lnG")
    lnB = S.sb(es, [64, 192], F32, "lnB")
    gmG = S.sb(es, [128, 128], F32, "gmG")
    gmB = S.sb(es, [128, 128], F32, "gmB")
    bsT = S.sb(es, [128, 2], F32, "bsT")
    WsT = S.sb(es, [128, 2, 128], BF16, "WsT")
    with ExitStack() as e0:
        s1 = S.sb(e0, [64, 192], F32, "s1")
        s2 = S.sb(e0, [64, 192], F32, "s2")
        s3 = S.sb(e0, [128, 192], F32, "s3")
        S.dma("sp", s1, A["w2"]); S.I("dve", "tensor_copy", out=w2b, in_=s1)
        S.dma("sp", s2, A["a2"]); S.I("dve", "tensor_copy", out=a2b, in_=s2)
        S.dma("sp", s3, A["g2"]); S.I("dve", "tensor_copy", out=g2b, in_=s3)
        S.dma("pool", lnG, A["lngb"][0].partition_broadcast(64))
        S.dma("pool", lnB, A["lngb"][1].partition_broadcast(64))
        S.dma("pool", gmG, A["gm_lngb"][0].partition_broadcast(128))
        S.dma("pool", gmB, A["gm_lngb"][1].partition_broadcast(128))
        S.dma("sp", bsT, A["bsT"])
        wsf = S.sb(e0, [128, 2, 128], F32, "wsf")
        wsm = S.sb(e0, [128, 2, 128], BF16, "wsm")
        for g in range(2):
            S.dma("sp", wsf[:, g, :], A["ws"][g])
            _tt(S, "dve", wsm[:, g, :], wsf[:, g, :], tril, MUL)
            S.I("pe", "transpose", out=B_tr[:, g * 128:(g + 1) * 128], in_=wsm[:, g, :], identity=ident)
        S.I("dve", "tensor_copy", out=WsT, in_=B_tr[:, 0:256].re("p (g i) -> p g i", g=2))
    S.barrier()
    import os
    MSTOP = int(os.environ.get('M_STOP', '99'))
    if MSTOP <= 1:
        return

    hnT = S.sb(es, [128, 16, BT], BF16, "hnT")
    xt = S.sb(es, [128, D], F32, "xt")
    hbf = S.sb(es, [128, D], BF16, "hbf")
    stt_ = S.sb(es, [128, 8], F32, "stt")
    raw = [S.sb(es, [128 if bi == 11 else 64, BT + 1], F32, "raw%d" % bi) for bi in range(12)]
    for r in raw:
        S.I("pool", "memset", ap=r[:, 0:1], constant=0.0)
    KT = S.sb(es, [64, 3, SEQ], BF16, "KT")
    Vt = S.sb(es, [128, 32, 3, 65], BF16, "Vt")
    S.I("pool", "memset", ap=Vt[:, :, :, 64:65], constant=1.0)
    Z = [S.sb(es, [64, 3, 64], BF16, "Z%d" % i) for i in range(2)]
    S.I("pool", "memset", ap=Z[0], constant=0.0)
    kmaxrun = S.sb(es, [3, 1], F32, "kmaxrun")
    S.I("pool", "memset", ap=kmaxrun, constant=0.0)
    tw = S.sb(es, [64, BT], BF16, "tw")
    alob = S.sb(es, [64, BT], BF16, "alob")
    sgl = S.sb(es, [128, BT], BF16, "sgl")
    AR = [S.sb(es, [64, NCH, 128], BF16, "AR%d" % h) for h in range(3)]
    BK = [S.sb(es, [64, NCH, 128], BF16, "BK%d" % h) for h in range(3)]
    KB = [S.sb(es, [64, NCH, 128], BF16, "KB%d" % h) for h in range(3)]
    DG = [S.sb(es, [64, NCH, 64], BF16, "DG%d" % h) for h in range(3)]
    vbf = [S.sb(es, [64, BT], BF16, "vbf%d" % h) for h in range(3)]
    prod = [S.sb(es, [64, BT], BF16, "prod%d" % h) for h in range(3)]
    GC = S.sb(es, [64, 8], F32, "GC")
    T = [S.sb(es, [64, BT], F32, "T%d" % i) for i in range(10)]
    TR = S.sb(es, [64, 9, 64], BF16, "TR")
    Pm = [S.sb(es, [64, 3, 128], BF16, "Pm%d" % i) for i in range(2)]
    PmT = [S.sb(es, [64, 3, 64], BF16, "PmT%d" % i) for i in range(2)]
    nPbT = S.sb(es, [64, 3, 64], BF16, "nPbT")
    QA2 = S.sb(es, [64, 3, 128], BF16, "QA2")
    Wsb = S.sb(es, [64, 3, 64], BF16, "Wsb")
    Usb = S.sb(es, [64, 3, 64], BF16, "Usb")
    yc = S.sb(es, [64, 3, 64], F32, "yc")
    ysq = S.sb(es, [64, 3, 64], F32, "ysq")
    yo = S.sb(es, [64, 192], F32, "yo")
    ys = S.sb(es, [64, 12], F32, "ys")
    qkf = S.sb(es, [128, 384], F32, "qkf")
    qkb = S.sb(es, [128, 384], BF16, "qkb")
    rt = S.sb(es, [128, 6, 8], F32, "rt")
    rt2 = S.sb(es, [128, 6, 8], F32, "rt2")
    QT = S.sb(es, [64, 3, 128], BF16, "QT")
    nrm6 = S.sb(es, [128, 6], F32, "nrm6")
    mx = S.sb(es, [3, 4], F32, "mx")
    dg3 = S.sb(es, [3, 3], F32, "dg3")
    negc = S.sb(es, [128, 3], F32, "negc")
    Pb = S.sb(es, [128, 512], BF16, "Pb")
    Pmk = S.sb(es, [128, 512], BF16, "Pmk")
    obt = S.sb(es, [128, 192], F32, "obt")
    rden = S.sb(es, [128, 3], F32, "rden")
    gu = S.sb(es, [128, 128], F32, "gu")
    gv = S.sb(es, [128, 128], F32, "gv")
    gvb = S.sb(es, [128, 128], BF16, "gvb")
    gst = S.sb(es, [128, 16], F32, "gst")
    uct = S.sb(es, [128, 128], F32, "uct")

    xv = A["x"].rearrange("(t p) d -> t p d", p=128)

    for blk in range(NBLK):
        tok0 = blk * BT
        for ti in range(NTI):
            S.dma("sp", xt, xv[blk * NTI + ti])
            _act(S, hbf, xt, AF.Square, accum_out=stt_[:, 0:1])
            rstd_from_ss(S, stt_[:, 0:1], stt_[:, 1:2], D, NORM_EPS)
            _act(S, hbf, xt, AF.Copy, scale=stt_[:, 1:2])
            for c0 in range(0, 16, 4):
                for j in range(4):
                    S.I("pe", "transpose", out=B_tr[:, j * 128:(j + 1) * 128],
                        in_=hbf[:, (c0 + j) * 128:(c0 + j + 1) * 128], identity=ident)
                S.I("dve", "tensor_copy", out=hnT[:, c0:c0 + 4, ti * 128:(ti + 1) * 128],
                    in_=B_tr[:, 0:512].re("p (c t) -> p c t", c=4))
        for bi in range(12):
            M = 128 if bi == 11 else 64
            off = bi * 64
            for c in range(16):
                S.I("pe", "matmul", inc=(c == 15), out=B_pf[0:M, 0:BT], lhsT=wfm[:, c, off:off + M], rhs=hnT[:, c, :],
                    start=(c == 0), stop=(c == 15))
            _act(S, raw[bi][:, 1:BT + 1], B_pf[0:M, 0:BT], AF.Copy)

        def tshift(bi, out, tmp):
            r = raw[bi]
            P = 128 if bi == 11 else 64
            _tt(S, "dve", tmp, r[:, 0:BT], r[:, 1:BT + 1], SUB)
            _stt(S, out, tmp, mu[0:P, bi:bi + 1], r[:, 1:BT + 1], MUL, ADD)
            S.I("pool", "tensor_copy", out=r[:, 0:1], in_=r[:, BT:BT + 1])

        tshift(9, T[0], T[9])
        _act(S, tw, T[0], AF.Tanh)
        tshift(10, alob, T[9])
        gl_tmp = xt[:, 0:BT]
        gl_tmp2 = xt[:, BT:2 * BT]
        tshift(11, gl_tmp2, gl_tmp)
        _act(S, sgl, gl_tmp2, AF.Sigmoid)
        for h in range(3):
            hc = slice(h * 64, (h + 1) * 64)
            v = lambda j: vec[:, 5 * h + j:5 * h + j + 1]
            r_s, k_s = T[0], T[1]
            tshift(3 * h + 0, r_s, T[9])
            tshift(3 * h + 1, k_s, T[9])
            tshift(3 * h + 2, vbf[h], T[9])
            S.I("pe", "matmul", out=B_pf[0:64, 0:BT], lhsT=w2b[:, hc], rhs=tw, start=True, stop=True)
            sg = T[2]
            _act(S, sg, B_pf[0:64, 0:BT], AF.Sigmoid, bias=v(0))
            lw = T[3]
            _ts(S, "pool", lw, sg, -DECAY_SCALE, MUL)
            S.I("pe", "matmul", out=B_pf[0:64, 0:BT], lhsT=a2b[:, hc], rhs=alob, start=True, stop=True)
            a_ = T[2]
            _act(S, a_, B_pf[0:64, 0:BT], AF.Sigmoid, bias=v(1))
            kx = T[4]
            _ts(S, "dve", kx, k_s, v(2), MUL)
            sq = T[5]
            _tt(S, "pool", sq, kx, kx, MUL)
            S.I("pe", "matmul", out=B_pf[0:64, 0:BT], lhsT=ones64f, rhs=sq, start=True, stop=True)
            nr = T[5]
            _act(S, nr, B_pf[0:64, 0:BT], AF.Sqrt)
            _ts(S, "dve", nr, nr, 1e-12, MAX)
            S.I("dve", "reciprocal", out=nr, in_=nr)
            kk = T[4]
            _tt(S, "dve", kk, kx, nr, MUL)
            tt_ = T[5]
            _ts(S, "dve", tt_, a_, v(3), MUL, vec[:, 15 + h:16 + h], ADD)
            km = T[1]
            _tt(S, "pool", km, k_s, tt_, MUL)
            beta = T[5]
            _tt(S, "pool", beta, kk, a_, MUL)
            _tt(S, "pool", prod[h], r_s, km, MUL)
            cs_ = T[6]
            S.I("dve", "tensor_tensor_scan", out=cs_, data0=V(ones64f.ap[:, 0:1].to_broadcast([64, BT]), ones64f.buf), data1=lw, initial=0.0, op0=MUL, op1=ADD)
            lcs = T[7]
            cs3 = cs_.re("p (c t) -> p c t", t=64)
            lcs3 = lcs.re("p (c t) -> p c t", t=64)
            S.I("dve", "tensor_copy", out=lcs3[:, 0, :], in_=cs3[:, 0, :])
            _tt(S, "dve", lcs3[:, 1:NCH, :], cs3[:, 1:NCH, :], cs3[:, 0:NCH - 1, 63:64].bc([64, NCH - 1, 64]), SUB)
            e1 = T[8]
            _tt(S, "dve", e1, lcs, lw, SUB)
            _act(S, e1, e1, AF.Exp)
            e2 = T[6]
            _act(S, e2, lcs, AF.Exp)
            e3 = T[3]
            _act(S, e3, lcs, AF.Exp, scale=-1.0)
            c3 = lambda t_: t_.re("p (c t) -> p c t", t=64)
            _tt(S, "pool", AR[h][:, :, 0:64], c3(e1), c3(kk), MUL)
            _tt(S, "pool", AR[h][:, :, 64:128], c3(e2), c3(r_s), MUL)
            _tt(S, "dve", BK[h][:, :, 0:64], c3(e3), c3(beta), MUL)
            _tt(S, "dve", BK[h][:, :, 64:128], c3(e3), c3(km), MUL)
            gch = T[9][:, 0:NCH]
            S.I("dve", "tensor_copy", out=gch, in_=c3(e2)[:, :, 63])
            gcb = gch.ap.unsqueeze(2).to_broadcast([64, NCH, 64])
            gcbV = V(gcb, gch.buf)
            _tt(S, "dve", KB[h][:, :, 0:64], BK[h][:, :, 64:128], gcbV, MUL)
            _stt(S, KB[h][:, :, 64:128], BK[h][:, :, 0:64], -1.0, gcbV, MUL, MUL)
            idb = V(identf.ap[0:64, 0:64].unsqueeze(1).to_broadcast([64, NCH, 64]), identf.buf)
            _tt(S, "dve", DG[h], idb, gcbV, MUL)

        if MSTOP <= 2:
            return
        for ti in range(NTI):
            qb = blk * NTI + ti
            t0 = tok0 + ti * 128
            for c in range(16):
                S.I("pe", "matmul", inc=(c == 15), out=B_g1, lhsT=hnT[:, c, ti * 128:(ti + 1) * 128], rhs=wtm[:, c, 0:512],
                    start=(c == 0), stop=(c == 15))
            for c in range(16):
                S.I("pe", "matmul", inc=(c == 15), out=B_g2[:, 0:320], lhsT=hnT[:, c, ti * 128:(ti + 1) * 128],
                    rhs=wtm[:, c, 512:832], start=(c == 0), stop=(c == 15))
            _act(S, gu, B_g1[:, 384:512], AF.Gelu_apprx_tanh)
            _act(S, gv, B_g2[:, 192:320], AF.Gelu_apprx_tanh)
            for g in range(2):
                gs = slice(g * 64, (g + 1) * 64)
                S.I("dve", "bn_stats", out=gst[:, g * 6:(g + 1) * 6], in_=gv[:, gs])
                S.I("dve", "bn_aggr", out=gst[:, 12 + 2 * g:14 + 2 * g], in_=gst[:, g * 6:(g + 1) * 6])
            for g in range(2):
                gs = slice(g * 64, (g + 1) * 64)
                var = gst[:, 13 + 2 * g:14 + 2 * g]
                _act(S, var, var, AF.Sqrt, bias=1e-5)
                S.I("dve", "reciprocal", out=var, in_=var)
                _ts(S, "dve", gv[:, gs], gv[:, gs], gst[:, 12 + 2 * g:13 + 2 * g], SUB, var, MUL)
            _tt(S, "dve", gv, gv, gmG, MUL)
            _tt(S, "dve", gvb, gv, gmB, ADD)
            for g in range(2):
                gs = slice(g * 64, (g + 1) * 64)
                S.I("pe", "matmul", out=B_g2[:, 320 + g * 64:384 + g * 64], lhsT=WsT[:, g, :], rhs=gvb[:, gs],
                    start=True, stop=True)
                _stt(S, uct[:, gs], B_g2[:, 320 + g * 64:384 + g * 64], bsT[:, g:g + 1], gu[:, gs], ADD, MUL)
            store("uc", t0, 128, uct)
            if MSTOP <= 3:
                return
            _act(S, qkf[:, 0:192], B_g1[:, 0:192], AF.Copy, scale=0.125)
            _act(S, qkf[:, 192:384], B_g1[:, 192:384], AF.Copy)
            S.I("dve", "tensor_copy", out=qkb, in_=qkf)
            S.I("dve", "tensor_copy", out=Vt[:, qb, :, 0:64], in_=B_g2[:, 0:192].re("p (h d) -> p h d", d=64))
            q3 = qkf.re("p (h d) -> p h d", d=64)
            qb3 = qkb.re("p (h d) -> p h d", d=64)
            cosb = V(cs.ap[:, qb, 0:8].unsqueeze(1).to_broadcast([128, 6, 8]), cs.buf)
            sinb = V(cs.ap[:, qb, 8:16].unsqueeze(1).to_broadcast([128, 6, 8]), cs.buf)
            x1, x2 = q3[:, :, 0:8], q3[:, :, 8:16]
            _tt(S, "dve", rt, x1, cosb, MUL)
            _tt(S, "dve", rt2, x2, sinb, MUL)
            _tt(S, "dve", qb3[:, :, 0:8], rt, rt2, SUB)
            _tt(S, "dve", rt, x2, cosb, MUL)
            _tt(S, "dve", rt2, x1, sinb, MUL)
            _tt(S, "dve", qb3[:, :, 8:16], rt, rt2, ADD)
            sq6 = V(Pb.ap[:, 0:384].rearrange("p (h d) -> p h d", d=64), Pb.buf)
            _tt(S, "dve", sq6, qb3, qb3, MUL)
            S.I("dve", "tensor_reduce", out=nrm6, in_=sq6, op=ADD, axis=AX.X)
            S.I("pe", "transpose", out=B_o[0:3, 256:384], in_=nrm6[:, 0:3], identity=identf)
            S.I("pe", "transpose", out=B_o[0:3, 384:512], in_=nrm6[:, 3:6], identity=identf)
            S.I("dve", "reduce_max", out=mx[:, 0:1], in_=B_o[0:3, 256:384], axis=AX.X)
            S.I("dve", "reduce_max", out=mx[:, 1:2], in_=B_o[0:3, 384:512], axis=AX.X)
            _tt(S, "dve", kmaxrun, kmaxrun, mx[:, 1:2], MAX)
            _tt(S, "dve", mx[:, 2:3], mx[:, 0:1], kmaxrun, MUL)
            _act(S, mx[:, 3:4], mx[:, 2:3], AF.Sqrt)
            _ts(S, "dve", dg3, identf[0:3, 0:3], mx[:, 3:4], MUL, -1.0, MUL)
            S.I("pe", "matmul", out=B_o[:, 200:203], lhsT=ones3, rhs=dg3, start=True, stop=True)
            S.I("dve", "tensor_copy", out=negc, in_=B_o[:, 200:203])
            trv = B_tr[0:64, 0:768].re("p (h t) -> p h t", h=6)
            for hh in range(6):
                S.I("pe", "transpose", out=trv[:, hh, :], in_=qkb[:, hh * 64:(hh + 1) * 64], identity=ident)
            S.I("dve", "tensor_copy", out=QT, in_=trv[:, 0:3, :])
            S.I("dve", "tensor_copy", out=KT[:, :, t0:t0 + 128], in_=trv[:, 3:6, :])
            kb0 = max(0, qb - 16)
            kbs = list(range(kb0, qb + 1))
            for h in range(3):
                ov = B_o[:, h * 65:(h + 1) * 65]
                for g0 in range(0, len(kbs), 4):
                    grp = kbs[g0:g0 + 4]
                    n = len(grp)
                    for j, kb in enumerate(grp):
                        S.I("pe", "matmul", out=B_s[:, j * 128:(j + 1) * 128], lhsT=KT[:, h, kb * 128:(kb + 1) * 128],
                            rhs=QT[:, h, :], start=True, stop=True)
                    _act(S, Pb[:, 0:n * 128], B_s[:, 0:n * 128], AF.Exp, bias=negc[:, h:h + 1])
                    j0 = 16 - (qb - grp[0])
                    _tt(S, "dve", Pmk[:, 0:n * 128], Pb[:, 0:n * 128],
                        amask[:, j0:j0 + n, :].re("p a b -> p (a b)"), MUL)
                    for j, kb in enumerate(grp):
                        S.I("pe", "matmul", inc=(kb == kbs[-1]), out=ov, lhsT=Pmk[:, j * 128:(j + 1) * 128],
                            rhs=Vt[:, kb, h, :], start=(kb == kbs[0]), stop=(kb == kbs[-1]))
                S.I("dve", "reciprocal", out=rden[:, h:h + 1], in_=B_o[:, h * 65 + 64:h * 65 + 65])
                _ts(S, "dve", obt[:, h * 64:(h + 1) * 64], B_o[:, h * 65:h * 65 + 64], rden[:, h:h + 1], MUL)
            store("ob", t0, 128, obt)
            if MSTOP <= 4:
                return
            for cc in range(2):
                c = ti * 2 + cc
                cs_sl = slice(c * 64, (c + 1) * 64)
                Zc, Zn = Z[c % 2], Z[(c + 1) % 2]
                trw = B_tr[0:64, 0:576].re("p (a b) -> p a b", a=9)
                for h in range(3):
                    S.I("pe", "transpose", out=trw[:, 3 * h + 0, :], in_=vbf[h][:, cs_sl], identity=ident[0:64, 0:64])
                    S.I("pe", "transpose", out=trw[:, 3 * h + 1, :], in_=KB[h][:, c, 0:64], identity=ident[0:64, 0:64])
                    S.I("pe", "transpose", out=trw[:, 3 * h + 2, :], in_=KB[h][:, c, 64:128], identity=ident[0:64, 0:64])
                S.I("dve", "tensor_copy", out=TR, in_=trw)
                r0v = R0[0:64, 0:384].re("p (h n) -> p h n", h=3)
                r1v = R1[0:64, 0:384].re("p (h n) -> p h n", h=3)
                for h in range(3):
                    S.I("pe", "matmul", out=r0v[:, h, :], lhsT=BK[h][:, c, 0:64], rhs=AR[h][:, c, :], start=True, stop=True)
                    S.I("pe", "matmul", out=r1v[:, h, :], lhsT=BK[h][:, c, 64:128], rhs=AR[h][:, c, :], start=True, stop=True)
                bc3 = lambda m_, n_: V(m_.ap.unsqueeze(1).to_broadcast([64, 3, n_]), m_.buf)
                _tt(S, "dve", Pm[0][:, :, 0:64], r0v[:, :, 0:64], bc3(maskN[:, 0:64], 64), MUL)
                _tt(S, "dve", nPbT, r0v[:, :, 64:128], bc3(maskN[:, 64:128], 64), MUL)
                _tt(S, "dve", QA2, r1v, bc3(maskP, 128), MUL)
                S.I("pool", "tensor_copy", out=Pm[0][:, :, 64:128], in_=bc3(ident[0:64, 0:64], 64))
                pbv = R0[0:64, 0:192].re("p (h n) -> p h n", h=3)
                for h in range(3):
                    S.I("pe", "matmul", out=pbv[:, h, :], lhsT=AR[h][:, c, 0:64], rhs=BK[h][:, c, 0:64], start=True, stop=True)
                _tt(S, "dve", PmT[0], pbv, bc3(maskL, 64), MUL)
                if MSTOP <= 5:
                    return
                lbv = R1[0:64, 0:192].re("p (h n) -> p h n", h=3)
                for j in range(1, 7):
                    pi, po = (j - 1) % 2, j % 2
                    for h in range(3):
                        S.I("pe", "matmul", out=r0v[:, h, :], lhsT=PmT[pi][:, h, :], rhs=Pm[pi][:, h, :], start=True, stop=True)
                    if j < 6:
                        for h in range(3):
                            S.I("pe", "matmul", out=lbv[:, h, :], lhsT=Pm[pi][:, h, 0:64], rhs=PmT[pi][:, h, :],
                                start=True, stop=True)
                        _act(S, Pm[po][:, :, 0:64], r0v[:, :, 0:64], AF.Copy)
                        _act(S, PmT[po], lbv, AF.Copy)
                    _tt(S, "dve", Pm[po][:, :, 64:128], r0v[:, :, 64:128], Pm[pi][:, :, 64:128], ADD)
                if MSTOP <= 6:
                    return
                Tm = Pm[0]
                pwv = R1[0:64, 192:384].re("p (h n) -> p h n", h=3)
                for h in range(3):
                    S.I("pe", "matmul", inc=False, out=pwv[:, h, :], lhsT=AR[h][:, c, 0:64], rhs=Zc[:, h, :], start=True, stop=False)
                    S.I("pe", "matmul", out=pwv[:, h, :], lhsT=QA2[:, h, 0:64], rhs=TR[:, 3 * h, :], start=False, stop=True)
                _act(S, Wsb, pwv, AF.Copy)
                puv = R1[0:64, 0:192].re("p (h n) -> p h n", h=3)
                for h in range(3):
                    S.I("pe", "matmul", out=puv[:, h, :], lhsT=Tm[:, h, 64:128], rhs=Wsb[:, h, :], start=True, stop=True)
                _act(S, Usb, puv, AF.Copy)
                if MSTOP <= 7:
                    return
                pyv = R0[0:64, 0:192].re("p (h n) -> p h n", h=3)
                pzv = R0[0:64, 192:384].re("p (h n) -> p h n", h=3)
                for h in range(3):
                    S.I("pe", "matmul", inc=False, out=pyv[:, h, :], lhsT=AR[h][:, c, 64:128], rhs=Zc[:, h, :], start=True, stop=False)
                    S.I("pe", "matmul", inc=False, out=pyv[:, h, :], lhsT=QA2[:, h, 64:128], rhs=TR[:, 3 * h, :], start=False, stop=False)
                    S.I("pe", "matmul", out=pyv[:, h, :], lhsT=nPbT[:, h, :], rhs=Usb[:, h, :], start=False, stop=True)
                for h in range(3):
                    S.I("pe", "matmul", inc=False, out=pzv[:, h, :], lhsT=DG[h][:, c, :], rhs=Zc[:, h, :], start=True, stop=False)
                    S.I("pe", "matmul", inc=False, out=pzv[:, h, :], lhsT=TR[:, 3 * h + 1, :], rhs=TR[:, 3 * h, :], start=False, stop=False)
                    S.I("pe", "matmul", out=pzv[:, h, :], lhsT=TR[:, 3 * h + 2, :], rhs=Usb[:, h, :], start=False, stop=True)
                for h in range(3):
                    S.I("pe", "matmul", out=R0[0:64, 384 + h:385 + h], lhsT=prod[h][:, cs_sl], rhs=rkb[:, h:h + 1], start=True, stop=True)
                S.I("pe", "matmul", out=R1[0:64, 192:384], lhsT=sgl[:, cs_sl], rhs=g2b, start=True, stop=True)
                _act(S, Zn, pzv, AF.Copy)
                if MSTOP <= 8:
                    return
                _act(S, yc, pyv, AF.Copy)
                S.I("dve", "tensor_reduce", out=ys[:, 0:3], in_=yc, op=ADD, axis=AX.X)
                _ts(S, "dve", ys[:, 0:3], ys[:, 0:3], 1.0 / 64.0, MUL)
                _tt(S, "dve", yc, yc, ys[:, 0:3].bc3(), SUB)
                _tt(S, "dve", ysq, yc, yc, MUL)
                S.I("dve", "tensor_reduce", out=ys[:, 3:6], in_=ysq, op=ADD, axis=AX.X)
                if MSTOP <= 9:
                    return
                _ts(S, "dve", ys[:, 3:6], ys[:, 3:6], 1.0 / 64.0, MUL, 64e-5, ADD)
                _act(S, ys[:, 6:9], ys[:, 3:6], AF.Sqrt)
                S.I("dve", "reciprocal", out=ys[:, 6:9], in_=ys[:, 6:9])
                _tt(S, "dve", yc, yc, ys[:, 6:9].bc3(), MUL)
                ycf = yc.re("p h n -> p (h n)")
                _tt(S, "dve", ycf, ycf, lnG, MUL)
                _tt(S, "dve", ycf, ycf, lnB, ADD)
                if MSTOP <= 10:
                    return
                S.I("dve", "tensor_copy", out=ys[:, 9:12], in_=R0[0:64, 384:387])
                for h in range(3):
                    _ts(S, "dve", ysq[:, h, :], TR[:, 3 * h, :], ys[:, 9 + h:10 + h], MUL)
                _tt(S, "dve", yc, yc, ysq, ADD)
                _tt(S, "dve", yo, ycf, R1[0:64, 192:384], MUL)
                if MSTOP <= 11:
                    return
                store("ya", tok0 + c * 64, 64, yo)


M_IN = [("x", [SEQ, D], F32), ("pos", [128, 32], I32), ("w_fm", [D, 832], F32), ("w_tm", [D, 832], F32),
        ("gT", [128, 16], F32), ("mu_fm", [128, 12], F32), ("vec", [64, 15], F32), ("w2", [64, 192], F32),
        ("a2", [64, 192], F32), ("g2", [128, 192], F32), ("lngb", [2, 192], F32), ("gm_lngb", [2, 128], F32),
        ("ws", [2, 128, 128], F32), ("bsT", [128, 2], F32), ("ident", [128, 128], BF16), ("identf", [128, 128], F32),
        ("amask", [128, 17, 128], BF16), ("rmask", [64, 448], F32), ("tril", [128, 128], F32)]


def build_M(NBLK=16):
    nc = bass.Bass("TRN2", target_bir_lowering=False)
    A = {n: nc.dram_tensor(n, sh, dt, kind="ExternalInput").ap() for (n, sh, dt) in M_IN}
    A["ya"] = nc.dram_tensor("ya", [SEQ, 192], F32, kind="ExternalOutput").ap()
    A["ob"] = nc.dram_tensor("ob", [SEQ, 192], F32, kind="ExternalOutput").ap()
    A["uc"] = nc.dram_tensor("uc", [SEQ, 128], F32, kind="ExternalOutput").ap()
    S = Sched(nc)
    with ExitStack() as es:
        Cst = m_consts_dev(S, es, A)
        m_phase(S, es, Cst, A, NBLK)
        S.finish()
    return nc, S


def m_consts():
    c = {}
    c["ident"] = np.eye(128, dtype=ml_dtypes.bfloat16)
    c["identf"] = np.eye(128, dtype=np.float32)
    ki = np.arange(128)[:, None, None]
    jj = np.arange(17)[None, :, None]
    qi = np.arange(128)[None, None, :]
    dist = 128 * (16 - jj) + qi - ki
    m = ((dist >= 0) & (dist <= 128)).astype(np.float32) + ((dist >= 0) & (dist <= 512) & (dist % 4 == 0)) \
        + ((dist >= 0) & (dist <= 2048) & (dist % 16 == 0))
    c["amask"] = m.astype(ml_dtypes.bfloat16)
    i = np.arange(64)[:, None]
    t = np.arange(64)[None, :]
    strict = (t > i).astype(np.float32)
    incl = (t >= i).astype(np.float32)
    lower = (t < i).astype(np.float32)
    c["rmask"] = np.concatenate([-strict, -incl, strict, incl, -lower, np.zeros((64, 128), np.float32)], 1)
    c["tril"] = np.tril(np.ones((128, 128), np.float32))
    return c


def m_inputs(inp, l, b, j, consts):
    hs = [3 * j + h for h in range(3)]
    w_in = inp["w_in"][l]
    cols = []
    for h in hs:
        for part in range(3):
            cols += list(range(part * 768 + h * 64, part * 768 + (h + 1) * 64))
    cols += list(range(2304, 2560))
    w_fm = np.ascontiguousarray(w_in[:, cols])
    mu = inp["rwkv_mu"][l][cols]
    mu_fm = np.zeros((128, 12), np.float32)
    for bi in range(11):
        mu_fm[:64, bi] = mu[bi * 64:(bi + 1) * 64]
    mu_fm[:, 11] = mu[704:832]
    A0, G0 = 2560, 2560 + 2304
    tc = []
    for part in range(2):
        for h in hs:
            tc += list(range(A0 + part * 768 + h * 64, A0 + part * 768 + (h + 1) * 64))
    gs = [2 * j, 2 * j + 1]
    for g in gs:
        tc += list(range(G0 + g * 64, G0 + (g + 1) * 64))
    for h in hs:
        tc += list(range(A0 + 1536 + h * 64, A0 + 1536 + (h + 1) * 64))
    for g in gs:
        tc += list(range(G0 + 512 + g * 64, G0 + 512 + (g + 1) * 64))
    w_tm = np.ascontiguousarray(w_in[:, tc])
    hc = []
    for h in hs:
        hc += list(range(h * 64, (h + 1) * 64))
    vec = np.zeros((64, 15), np.float32)
    for k, h in enumerate(hs):
        sl = slice(h * 64, (h + 1) * 64)
        vec[:, 5 * k + 0] = inp["rwkv_w0"][l][sl]
        vec[:, 5 * k + 1] = inp["rwkv_a0"][l][sl]
        vec[:, 5 * k + 2] = inp["rwkv_k_k"][l][sl]
        vec[:, 5 * k + 3] = inp["rwkv_k_a"][l][sl]
        vec[:, 5 * k + 4] = inp["rwkv_r_k"][l][h]
    gc = []
    for g in gs:
        gc += list(range(g * 64, (g + 1) * 64))
    m = dict(consts)
    m.update({
        "x": np.ascontiguousarray(inp["x_cur"][b]),
        "pos": np.ascontiguousarray(np.asarray(inp["positions"][b]).reshape(32, 128).T.astype(np.int32)),
        "w_fm": w_fm, "w_tm": w_tm,
        "gT": np.ascontiguousarray(np.asarray(inp["norm_mix_pre"][l]).reshape(16, 128).T),
        "mu_fm": mu_fm, "vec": vec,
        "w2": np.ascontiguousarray(inp["rwkv_w2"][l][:, hc]), "a2": np.ascontiguousarray(inp["rwkv_a2"][l][:, hc]),
        "g2": np.ascontiguousarray(inp["rwkv_g2"][l][:, hc]),
        "lngb": np.ascontiguousarray(np.stack([inp["rwkv_ln_g"][l][hc], inp["rwkv_ln_b"][l][hc]])),
        "gm_lngb": np.ascontiguousarray(np.stack([inp["gmlp_ln_g"][l][gc], inp["gmlp_ln_b"][l][gc]])),
        "ws": np.ascontiguousarray(inp["gmlp_ws"][l][gs]),
        "bsT": np.ascontiguousarray(inp["gmlp_bs"][l][gs].T),
    })
    return m


_PROGS = {}


def _prog(name):
    if name not in _PROGS:
        _PROGS[name] = build_M()[0] if name == "M" else build_T(512, 2)[0]
    return _PROGS[name]


def kernel(**inputs):
    import time
    inp = {k: np.asarray(v) for k, v in inputs.items()}
    x = np.ascontiguousarray(inp["x"], dtype=np.float32)
    B = x.shape[0]
    consts = m_consts()
    cores = list(range(8))
    for l in range(4):
        t0_ = time.time()
        inp["x_cur"] = x
        in_maps = [m_inputs(inp, l, c // 4, c % 4, consts) for c in cores]
        res = run_bass_kernel_spmd(_prog("M"), in_maps, core_ids=cores).results
        y = np.zeros((B, SEQ, D), np.float32)
        for c in cores:
            b, j = c // 4, c % 4
            y[b, :, j * 192:(j + 1) * 192] = res[c]["ya"]
            y[b, :, 768 + j * 192:768 + (j + 1) * 192] = res[c]["ob"]
            y[b, :, 1536 + j * 128:1536 + (j + 1) * 128] = res[c]["uc"]
        t1_ = time.time()
        Wt = t_weights(inp, l)
        in_maps = []
        for c in cores:
            b, j = c // 4, c % 4
            m = dict(Wt)
            m["xin"] = t_shard(x, b, j * 1024, 512, 2)
            m["yin"] = t_shard(y, b, j * 1024, 512, 2)
            in_maps.append(m)
        res = run_bass_kernel_spmd(_prog("T"), in_maps, core_ids=cores).results
        xn = np.empty_like(x)
        for c in cores:
            b, j = c // 4, c % 4
            xn[b, j * 1024:(j + 1) * 1024] = res[c]["xout"].reshape(1024, D)
        x = xn
        print("layer", l, "M %.1fs T %.1fs" % (t1_ - t0_, time.time() - t1_), flush=True)
    return x


RG = [[0, 1, 2, 3], [4, 5, 6, 7]]
M_LAYER = [(n, sh, dt) for (n, sh, dt) in M_IN if n not in ("x", "pos", "ident", "identf", "amask", "rmask", "tril")]
T_LAYER = [("w_out", [D, D]), ("ffn_up", [D, 2 * DFF]), ("ffn_down", [DFF, D]), ("cwb", [128, 88, 4]),
           ("g_post", [D]), ("g_pre2", [D]), ("g_post2", [D]), ("attn_g", [768]), ("gmlp_g", [512])]


def build_fused(NL=4):
    nc = bass.Bass("TRN2", target_bir_lowering=False)
    A = {}

    def din(name, shape, dt=F32):
        A[name] = nc.dram_tensor(name, list(shape), dt, kind="ExternalInput").ap()

    din("x_my", [1024, D]); din("x_full0", [SEQ, D]); din("sel", [128, 7]); din("pos", [128, 32], I32)
    for (n, sh, dt) in M_IN:
        if n in ("ident", "identf", "amask", "rmask", "tril"):
            din(n, sh, dt)
    for (n, sh, dt) in M_LAYER:
        din(n, [NL] + list(sh), dt)
    for (n, sh) in T_LAYER:
        din(n, [NL] + list(sh), F32)
    xout = nc.dram_tensor("xout", [1024, D], F32, kind="ExternalOutput").ap()
    YR = 128 + SEQ
    ysend = [nc.dram_tensor("ysend%d" % i, [YR, 512], F32, kind="Internal").ap() for i in range(2)]
    ygat = [nc.dram_tensor("ygat%d" % i, [4 * YR, 512], F32, kind="Internal").ap() for i in range(2)]
    xsh = [nc.dram_tensor("xsh%d" % i, [1024, D], F32, kind="Internal").ap() for i in range(2)]
    xfull = [nc.dram_tensor("xfull%d" % i, [SEQ, D], F32, kind="Internal").ap() for i in range(2)]
    x1_scr = nc.dram_tensor("x1_scr", [512, D], F32, kind="Internal").ap()
    S = Sched(nc)
    csem = [nc.alloc_semaphore("cc%d" % i) for i in range(2 * NL)]
    for s_ in csem:
        nc.gpsimd.sem_clear(s_)
    nc.all_engine_barrier()

    def cc(idx, src, dst):
        S.barrier()
        ins = nc.gpsimd.collective_compute("AllGather", ALU.bypass, replica_groups=RG, ins=[src], outs=[dst])
        ins.then_inc(csem[idx], 16)
        tok = Tok("dma", csem[idx], ("cc", idx), 16)
        for eng in ENGS:
            S._wait(eng, tok)

    with ExitStack() as es:
        Cst = m_consts_dev(S, es, A)
        sel = S.sb(es, [128, 7], F32, "sel")
        S.dma("sp", sel, A["sel"])
        zt = S.sb(es, [128, 512], F32, "zt")
        S.I("dve", "memset", ap=zt, constant=0.0)
        for i in range(2):
            S.dma("sp", ysend[i][0:128, :], zt)
        C = {"ident": Cst["ident"]}
        for l in range(NL):
            xsrc = A["x_full0"] if l == 0 else xfull[(l - 1) % 2]
            xmy = A["x_my"] if l == 0 else xsh[(l - 1) % 2]
            ys, yg = ysend[l % 2], ygat[l % 2]
            Al = {n: A[n][l] for (n, sh, dt) in M_LAYER}
            Al["x"] = xsrc

            def store(kind, t0, n, tile, ys=ys):
                c0, c1 = {"ya": (0, 192), "ob": (192, 384), "uc": (384, 512)}[kind]
                S.dma("sp", ys[128 + t0:128 + t0 + n, c0:c1], tile)

            with ExitStack() as em:
                m_phase(S, em, Cst, Al, store=store)
            cc(2 * l, ys, yg)
            W = {n: A[n][l] for (n, sh) in T_LAYER}
            yg4 = yg.rearrange("(r t) c -> t r c", r=4)
            for p in range(2):
                def load_x(i, xt, scratch, p=p, xmy=xmy, xsrc=xsrc):
                    if i >= 1:
                        S.dma("sp", xt, xmy[p * 512 + (i - 1) * 128:p * 512 + i * 128, :])
                    elif p == 1:
                        S.dma("sp", xt, xmy[384:512, :])
                    else:
                        for r in range(3):
                            sc = scratch[r % 2]
                            S.dma("sp", sc, xsrc[r * 1024 + 896:(r + 1) * 1024, :])
                            if r == 0:
                                _ts(S, "dve", xt, sc, sel[:, 0:1], ALU.mult)
                            else:
                                _stt(S, xt, sc, sel[:, r:r + 1], xt, ALU.mult, ALU.add)

                def load_y(i, yc, scratch, p=p, yg4=yg4):
                    for jj in range(4):
                        sc = scratch[jj % 2]
                        r0_ = jj * 1024 + p * 512 + i * 128
                        S.dma("sp", sc.re("p (r c) -> p r c", r=4), yg4[r0_:r0_ + 128, :, :])
                        if jj == 0:
                            _ts(S, "dve", yc, sc, sel[:, 3:4], ALU.mult)
                        else:
                            _stt(S, yc, sc, sel[:, 3 + jj:4 + jj], yc, ALU.mult, ALU.add)

                xo = (xout if l == NL - 1 else xsh[l % 2])[p * 512:(p + 1) * 512, :]
                t_pass(S, es, C, W, None, None, xo, x1_scr, 512, load_x, load_y, perm=True)
            if l < NL - 1:
                cc(2 * l + 1, xsh[l % 2], xfull[l % 2])
        S.barrier()
        nc.all_engine_barrier()
        for s_ in csem:
            nc.gpsimd.sem_clear(s_)
        S.finish()
    return nc, S


YPERM = []
for _r in range(4):
    YPERM += list(range(_r * 192, (_r + 1) * 192)) + list(range(768 + _r * 192, 768 + (_r + 1) * 192)) \
        + list(range(1536 + _r * 128, 1536 + (_r + 1) * 128))


def fused_inputs(inp, consts, NL=4):
    x = np.ascontiguousarray(inp["x"], dtype=np.float32)
    inp = dict(inp)
    inp["x_cur"] = x
    tw = [t_weights(inp, l) for l in range(NL)]
    shared = {n: np.ascontiguousarray(np.stack([tw[l][n] for l in range(NL)])) for (n, sh) in T_LAYER}
    shared["w_out"] = np.ascontiguousarray(shared["w_out"][:, YPERM, :])
    maps = []
    for c in range(8):
        b, j = c // 4, c % 4
        ml = [m_inputs(inp, l, b, j, consts) for l in range(NL)]
        m = dict(shared)
        for (n, sh, dt) in M_LAYER:
            m[n] = np.ascontiguousarray(np.stack([ml[l][n] for l in range(NL)]))
        for n in ("ident", "identf", "amask", "rmask", "tril"):
            m[n] = consts[n]
        m["pos"] = ml[0]["pos"]
        m["x_my"] = np.ascontiguousarray(x[b, j * 1024:(j + 1) * 1024])
        m["x_full0"] = np.ascontiguousarray(x[b])
        sel = np.zeros((128, 7), np.float32)
        if j > 0:
            sel[:, j - 1] = 1.0
        sel[:, 3 + j] = 1.0
        m["sel"] = sel
        maps.append(m)
    return maps


def kernel_fused(**inputs):
    inp = {k: np.asarray(v) for k, v in inputs.items()}
    if "F" not in _PROGS:
        _PROGS["F"] = build_fused()[0]
    maps = fused_inputs(inp, m_consts())
    res = run_bass_kernel_spmd(_PROGS["F"], maps, core_ids=list(range(8))).results
    B = inp["x"].shape[0]
    out = np.empty((B, SEQ, D), np.float32)
    for c in range(8):
        b, j = c // 4, c % 4
        out[b, j * 1024:(j + 1) * 1024] = res[c]["xout"]
    return out
```
